# Optimizing a Trainium2 kernel written in Bass

```python
import math
import jax, jax.numpy as jnp
from jax import lax
import numpy as np

D_MODEL = 4096
BATCH = 4
SEQ = 2048
DEPTH = 2

CHUNK = 64
N_EVEN = (DEPTH + 1) // 2
N_ODD = DEPTH // 2
DEEPNORM_ALPHA = (2 * DEPTH) ** 0.25
DEEPNORM_BETA = (8 * DEPTH) ** -0.25
LN_EPS = 1e-5
RMS_EPS = 1e-6

MIX_WIDTH = D_MODEL
GM_WIDTH = MIX_WIDTH // 2
GM_GROUPS = 8
GM_GROUP_DIM = GM_WIDTH // GM_GROUPS
GM_BLOCK = 128
SSM_WIDTH = MIX_WIDTH - GM_WIDTH
SSM_GROUP_DIM = 16
SSM_GROUPS = SSM_WIDTH // SSM_GROUP_DIM
SSM_STATE = 64
HY_IN_WIDTH = 2 * GM_WIDTH + SSM_WIDTH
DT_MIN = 1e-3
DT_MAX = 1e-1

MLA_HEADS = 32
MLA_Q_RANK = 1024
MLA_KV_RANK = 512
MLA_NOPE = 128
MLA_ROPE = 64
MLA_V = 128
MLA_IN_WIDTH = MLA_Q_RANK + MLA_KV_RANK + MLA_ROPE
ROPE_THETA = 10000.0
Q_BLOCK = 128

N_EXPERTS = 32
TOP_K = 4
D_EXPERT = D_MODEL // 4
SWIGLU_LIMIT = 7.0
SWIGLU_ALPHA = 1.702
MOE_BLOCK = 128

kernel_name = "hybrid_gmlp_s5_mla_moe_deepnorm"


def layer_norm(x, g, b):
    xf = x.astype(jnp.float32)
    mu = jnp.mean(xf, -1, keepdims=True)
    var = jnp.mean(jnp.square(xf - mu), -1, keepdims=True)
    return ((xf - mu) * lax.rsqrt(var + LN_EPS) * g + b).astype(x.dtype)


def rms_norm(x, g):
    xf = x.astype(jnp.float32)
    return (xf * lax.rsqrt(jnp.mean(xf * xf, -1, keepdims=True) + RMS_EPS) * g).astype(x.dtype)


def chunk_causal_mask(q_idx, k_idx):
    return (k_idx[None, :] // CHUNK) <= (q_idx[:, None] // CHUNK)


def chunked_spatial_gating(z, ln_g, ln_b, w_s, b_s):
    bsz, seq, _ = z.shape
    u, v = jnp.split(z, 2, axis=-1)
    nblk = seq // GM_BLOCK
    v = v.reshape(bsz, nblk, GM_BLOCK, GM_GROUPS, GM_GROUP_DIM)
    v = layer_norm(v, ln_g, ln_b)
    idx = jnp.arange(GM_BLOCK)
    w = jnp.where(chunk_causal_mask(idx, idx)[None], w_s, 0.0)
    s = jnp.einsum('gij,bnjgc->bnigc', w, v) + b_s.T[:, :, None]
    return u * s.reshape(bsz, seq, GM_WIDTH)


def s5_mixer(u, lam_re, lam_im, log_dt, b_re, b_im, c_re, c_im, d_skip, w_glu, b_glu):
    f32 = jnp.float32
    bsz, seq, _ = u.shape
    uf = u.astype(f32).reshape(bsz, seq, SSM_GROUPS, SSM_GROUP_DIM)
    lam_re = lam_re.astype(f32)
    lam_im = lam_im.astype(f32)
    b_re = b_re.astype(f32)
    b_im = b_im.astype(f32)
    dt = jnp.exp(log_dt.astype(f32))[:, None]
    mag = jnp.exp(lam_re * dt)
    ab_re = mag * jnp.cos(lam_im * dt)
    ab_im = mag * jnp.sin(lam_im * dt)
    den = lam_re * lam_re + lam_im * lam_im
    num_re = ab_re - 1.0
    coef_re = (num_re * lam_re + ab_im * lam_im) / den
    coef_im = (ab_im * lam_re - num_re * lam_im) / den
    bb_re = coef_re[..., None] * b_re - coef_im[..., None] * b_im
    bb_im = coef_re[..., None] * b_im + coef_im[..., None] * b_re
    x_re = jnp.einsum('gnp,bsgp->bsgn', bb_re, uf)
    x_im = jnp.einsum('gnp,bsgp->bsgn', bb_im, uf)
    a_re = jnp.broadcast_to(ab_re[None, None], (1, seq, SSM_GROUPS, SSM_STATE))
    a_im = jnp.broadcast_to(ab_im[None, None], (1, seq, SSM_GROUPS, SSM_STATE))

    def combine(e1, e2):
        a1r, a1i, b1r, b1i = e1
        a2r, a2i, b2r, b2i = e2
        return (a2r * a1r - a2i * a1i,
                a2r * a1i + a2i * a1r,
                a2r * b1r - a2i * b1i + b2r,
                a2r * b1i + a2i * b1r + b2i)

    _, _, h_re, h_im = lax.associative_scan(combine, (a_re, a_im, x_re, x_im), axis=1)
    y = (jnp.einsum('gpn,bsgn->bsgp', c_re.astype(f32), h_re)
         - jnp.einsum('gpn,bsgn->bsgp', c_im.astype(f32), h_im)
         + d_skip.astype(f32) * uf)
    y = jax.nn.gelu(y.reshape(bsz, seq, SSM_WIDTH)).astype(u.dtype)
    return y * jax.nn.sigmoid(y @ w_glu + b_glu)


def apply_rope(x, cos, sin):
    x1, x2 = jnp.split(x, 2, axis=-1)
    return jnp.concatenate([x1 * cos - x2 * sin, x2 * cos + x1 * sin], axis=-1)


def mla_mixer(x, positions, w_in, q_norm_g, kv_norm_g, w_uq, w_ukv, w_o):
    bsz, seq, _ = x.shape
    c = x @ w_in
    cq, ckv, k_rope = jnp.split(c, [MLA_Q_RANK, MLA_Q_RANK + MLA_KV_RANK], axis=-1)
    q = (rms_norm(cq, q_norm_g) @ w_uq).reshape(bsz, seq, MLA_HEADS, MLA_NOPE + MLA_ROPE)
    kv = (rms_norm(ckv, kv_norm_g) @ w_ukv).reshape(bsz, seq, MLA_HEADS, MLA_NOPE + MLA_V)
    q_nope, q_rope = jnp.split(q, [MLA_NOPE], axis=-1)
    k_nope, v = jnp.split(kv, [MLA_NOPE], axis=-1)
    inv_freq = ROPE_THETA ** (-jnp.arange(0, MLA_ROPE, 2, dtype=jnp.float32) / MLA_ROPE)
    ang = positions.astype(jnp.float32)[..., None] * inv_freq
    cos, sin = jnp.cos(ang), jnp.sin(ang)
    q_rope = apply_rope(q_rope.astype(jnp.float32), cos[:, :, None], sin[:, :, None]).astype(x.dtype)
    k_rope = apply_rope(k_rope.astype(jnp.float32), cos, sin).astype(x.dtype)
    scale = (MLA_NOPE + MLA_ROPE) ** -0.5
    outs = []
    for blk in range(seq // Q_BLOCK):
        q0, q1 = blk * Q_BLOCK, (blk + 1) * Q_BLOCK
        s = (jnp.einsum('bqhd,bkhd->bhqk', q_nope[:, q0:q1], k_nope[:, :q1])
             + jnp.einsum('bqhr,bkr->bhqk', q_rope[:, q0:q1], k_rope[:, :q1]))
        s = s.astype(jnp.float32) * scale
        mask = chunk_causal_mask(jnp.arange(q0, q1), jnp.arange(q1))
        p = jax.nn.softmax(jnp.where(mask, s, -jnp.inf), axis=-1).astype(v.dtype)
        outs.append(jnp.einsum('bhqk,bkhd->bqhd', p, v[:, :q1]))
    o = jnp.concatenate(outs, axis=1).reshape(bsz, seq, MLA_HEADS * MLA_V)
    return o @ w_o


def moe_ffn(x, w_router, b_router, w_gu, b_gu, w_down, b_down):
    bsz, seq, d = x.shape
    n_tok = bsz * seq
    xt = x.reshape(n_tok, d)
    logits = (xt @ w_router).astype(jnp.float32) + b_router
    top_val, top_idx = lax.top_k(logits, TOP_K)
    gates = jax.nn.softmax(top_val, axis=-1)
    n_assign = n_tok * TOP_K
    flat_e = top_idx.reshape(-1)
    flat_tok = jnp.arange(n_assign, dtype=jnp.int32) // TOP_K
    flat_gate = gates.reshape(-1)
    counts = jnp.bincount(flat_e, length=N_EXPERTS)
    padded = (counts + MOE_BLOCK - 1) // MOE_BLOCK * MOE_BLOCK
    start = jnp.cumsum(counts) - counts
    pad_end = jnp.cumsum(padded)
    pad_start = pad_end - padded
    order = jnp.argsort(flat_e)
    sorted_e = flat_e[order]
    dest = pad_start[sorted_e] + jnp.arange(n_assign, dtype=jnp.int32) - start[sorted_e]
    n_rows = n_assign + N_EXPERTS * MOE_BLOCK
    n_blocks = n_rows // MOE_BLOCK
    row_tok = jnp.full((n_rows,), n_tok, jnp.int32).at[dest].set(flat_tok[order])
    row_gate = jnp.zeros((n_rows,), jnp.float32).at[dest].set(flat_gate[order])
    blk_exp = jnp.minimum(
        jnp.searchsorted(pad_end, jnp.arange(n_blocks) * MOE_BLOCK, side='right'), N_EXPERTS - 1)
    x_pad = jnp.concatenate([xt, jnp.zeros((1, d), xt.dtype)], axis=0)

    def expert_block(args):
        e, tok, g = args
        h = x_pad[tok] @ w_gu[e] + b_gu[e]
        h_glu, h_lin = jnp.split(h, 2, axis=-1)
        h_glu = jnp.minimum(h_glu, SWIGLU_LIMIT)
        h_lin = jnp.clip(h_lin, -SWIGLU_LIMIT, SWIGLU_LIMIT)
        act = h_glu * jax.nn.sigmoid(SWIGLU_ALPHA * h_glu) * (h_lin + 1.0)
        y = act @ w_down[e] + b_down[e]
        return y * g[:, None].astype(y.dtype)

    y_rows = lax.map(expert_block, (blk_exp, row_tok.reshape(n_blocks, MOE_BLOCK),
                                    row_gate.reshape(n_blocks, MOE_BLOCK)))
    y = jnp.zeros((n_tok + 1, d), x.dtype).at[row_tok].add(y_rows.reshape(n_rows, d).astype(x.dtype))
    return y[:n_tok].reshape(bsz, seq, d)


def setup_inputs(seed: int = 0) -> dict:
    key = jax.random.key(seed)
    k = jax.random.split(key, 32)
    f32 = jnp.float32

    def nrm(kk, shape, scale):
        return jax.random.normal(kk, shape, f32) * scale

    x = nrm(k[0], (BATCH, SEQ, D_MODEL), 1.0)
    offsets = jax.random.randint(k[1], (BATCH, 1), 0, 64) * CHUNK
    positions = (offsets + jnp.arange(SEQ)[None, :]).astype(jnp.int32)
    ln_g = 1.0 + nrm(k[2], (DEPTH, 2, D_MODEL), 0.02)
    ln_b = nrm(k[3], (DEPTH, 2, D_MODEL), 0.02)
    hy_w_in = nrm(k[4], (N_EVEN, D_MODEL, HY_IN_WIDTH), D_MODEL ** -0.5)
    hy_w_out = nrm(k[5], (N_EVEN, MIX_WIDTH, D_MODEL), MIX_WIDTH ** -0.5 * DEEPNORM_BETA)
    gm_ln_g = 1.0 + nrm(k[6], (N_EVEN, GM_GROUPS, GM_GROUP_DIM), 0.02)
    gm_ln_b = nrm(k[7], (N_EVEN, GM_GROUPS, GM_GROUP_DIM), 0.02)
    gm_w_s = nrm(k[8], (N_EVEN, GM_GROUPS, GM_BLOCK, GM_BLOCK), GM_BLOCK ** -0.5)
    gm_b_s = 1.0 + nrm(k[9], (N_EVEN, GM_GROUPS, GM_BLOCK), 0.02)
    ssm_lam_re = -0.5 + nrm(k[10], (N_EVEN, SSM_GROUPS, SSM_STATE), 0.01)
    ssm_lam_im = (jnp.pi * jnp.arange(SSM_STATE, dtype=f32))[None, None] + nrm(
        k[11], (N_EVEN, SSM_GROUPS, SSM_STATE), 0.01)
    ssm_log_dt = jax.random.uniform(k[12], (N_EVEN, SSM_GROUPS), f32,
                                    minval=math.log(DT_MIN), maxval=math.log(DT_MAX))
    ssm_b_re = nrm(k[13], (N_EVEN, SSM_GROUPS, SSM_STATE, SSM_GROUP_DIM), (2 * SSM_GROUP_DIM) ** -0.5)
    ssm_b_im = nrm(k[14], (N_EVEN, SSM_GROUPS, SSM_STATE, SSM_GROUP_DIM), (2 * SSM_GROUP_DIM) ** -0.5)
    ssm_c_re = nrm(k[15], (N_EVEN, SSM_GROUPS, SSM_GROUP_DIM, SSM_STATE), SSM_STATE ** -0.5)
    ssm_c_im = nrm(k[16], (N_EVEN, SSM_GROUPS, SSM_GROUP_DIM, SSM_STATE), SSM_STATE ** -0.5)
    ssm_d = nrm(k[17], (N_EVEN, SSM_GROUPS, SSM_GROUP_DIM), 1.0)
    ssm_w_glu = nrm(k[18], (N_EVEN, SSM_WIDTH, SSM_WIDTH), SSM_WIDTH ** -0.5)
    ssm_b_glu = nrm(k[19], (N_EVEN, SSM_WIDTH), 0.02)
    mla_w_in = nrm(k[20], (N_ODD, D_MODEL, MLA_IN_WIDTH), D_MODEL ** -0.5)
    mla_q_norm_g = 1.0 + nrm(k[21], (N_ODD, MLA_Q_RANK), 0.02)
    mla_kv_norm_g = 1.0 + nrm(k[22], (N_ODD, MLA_KV_RANK), 0.02)
    mla_w_uq = nrm(k[23], (N_ODD, MLA_Q_RANK, MLA_HEADS * (MLA_NOPE + MLA_ROPE)), MLA_Q_RANK ** -0.5)
    mla_w_ukv = nrm(k[24], (N_ODD, MLA_KV_RANK, MLA_HEADS * (MLA_NOPE + MLA_V)), MLA_KV_RANK ** -0.5)
    mla_w_o = nrm(k[25], (N_ODD, MLA_HEADS * MLA_V, D_MODEL), (MLA_HEADS * MLA_V) ** -0.5 * DEEPNORM_BETA)
    moe_w_router = nrm(k[26], (DEPTH, D_MODEL, N_EXPERTS), D_MODEL ** -0.5)
    moe_b_router = nrm(k[27], (DEPTH, N_EXPERTS), 0.01)
    moe_w_gu = nrm(k[28], (DEPTH, N_EXPERTS, D_MODEL, 2 * D_EXPERT), D_MODEL ** -0.5)
    moe_b_gu = nrm(k[29], (DEPTH, N_EXPERTS, 2 * D_EXPERT), 0.02)
    moe_w_down = nrm(k[30], (DEPTH, N_EXPERTS, D_EXPERT, D_MODEL), D_EXPERT ** -0.5 * DEEPNORM_BETA)
    moe_b_down = nrm(k[31], (DEPTH, N_EXPERTS, D_MODEL), 0.02 * DEEPNORM_BETA)
    return {
        "x": x, "positions": positions, "ln_g": ln_g, "ln_b": ln_b,
        "hy_w_in": hy_w_in, "hy_w_out": hy_w_out,
        "gm_ln_g": gm_ln_g, "gm_ln_b": gm_ln_b, "gm_w_s": gm_w_s, "gm_b_s": gm_b_s,
        "ssm_lam_re": ssm_lam_re, "ssm_lam_im": ssm_lam_im, "ssm_log_dt": ssm_log_dt,
        "ssm_b_re": ssm_b_re, "ssm_b_im": ssm_b_im, "ssm_c_re": ssm_c_re, "ssm_c_im": ssm_c_im,
        "ssm_d": ssm_d, "ssm_w_glu": ssm_w_glu, "ssm_b_glu": ssm_b_glu,
        "mla_w_in": mla_w_in, "mla_q_norm_g": mla_q_norm_g, "mla_kv_norm_g": mla_kv_norm_g,
        "mla_w_uq": mla_w_uq, "mla_w_ukv": mla_w_ukv, "mla_w_o": mla_w_o,
        "moe_w_router": moe_w_router, "moe_b_router": moe_b_router,
        "moe_w_gu": moe_w_gu, "moe_b_gu": moe_b_gu, "moe_w_down": moe_w_down, "moe_b_down": moe_b_down,
    }


def reference(x, positions, ln_g, ln_b, hy_w_in, hy_w_out, gm_ln_g, gm_ln_b, gm_w_s, gm_b_s,
              ssm_lam_re, ssm_lam_im, ssm_log_dt, ssm_b_re, ssm_b_im, ssm_c_re, ssm_c_im,
              ssm_d, ssm_w_glu, ssm_b_glu, mla_w_in, mla_q_norm_g, mla_kv_norm_g, mla_w_uq,
              mla_w_ukv, mla_w_o, moe_w_router, moe_b_router, moe_w_gu, moe_b_gu,
              moe_w_down, moe_b_down):
    h = x
    for layer in range(DEPTH):
        i = layer // 2
        if layer % 2 == 0:
            z = h @ hy_w_in[i]
            z_gm, z_ssm = jnp.split(z, [2 * GM_WIDTH], axis=-1)
            y_gm = chunked_spatial_gating(jax.nn.gelu(z_gm), gm_ln_g[i], gm_ln_b[i],
                                          gm_w_s[i], gm_b_s[i])
            y_ssm = s5_mixer(z_ssm, ssm_lam_re[i], ssm_lam_im[i], ssm_log_dt[i],
                             ssm_b_re[i], ssm_b_im[i], ssm_c_re[i], ssm_c_im[i],
                             ssm_d[i], ssm_w_glu[i], ssm_b_glu[i])
            mix = jnp.concatenate([y_gm, y_ssm], axis=-1) @ hy_w_out[i]
        else:
            mix = mla_mixer(h, positions, mla_w_in[i], mla_q_norm_g[i], mla_kv_norm_g[i],
                            mla_w_uq[i], mla_w_ukv[i], mla_w_o[i])
        h = layer_norm(DEEPNORM_ALPHA * h + mix, ln_g[layer, 0], ln_b[layer, 0])
        ffn = moe_ffn(h, moe_w_router[layer], moe_b_router[layer], moe_w_gu[layer],
                      moe_b_gu[layer], moe_w_down[layer], moe_b_down[layer])
        h = layer_norm(DEEPNORM_ALPHA * h + ffn, ln_g[layer, 1], ln_b[layer, 1])
    return h
```

```python
import math
from contextlib import ExitStack
import numpy as np
import concourse.bass as bass
import concourse.mybir as mybir
from concourse.bass_utils import run_bass_kernel_spmd

F32 = mybir.dt.float32
BF16 = mybir.dt.bfloat16
I32 = mybir.dt.int32
U32 = mybir.dt.uint32
ALU = mybir.AluOpType
AF = mybir.ActivationFunctionType
AX = mybir.AxisListType

D = 4096
NT = 1024
NTILE = NT // 128
ALPHA = 4 ** 0.25
LN_EPS = 1e-5
RMS_EPS = 1e-6
NEXP = 32
DEXP = 1024
TWO_PI = 2.0 * math.pi
CW1 = 6.28125
CW2 = TWO_PI - 6.28125
MAGIC = 12582912.0


def K(ap):
    return ap.tensor.name


class StopBuild(Exception):
    pass


class Prog:
    def __init__(self, nc, stack):
        self.nc = nc
        self.root = stack
        self.stack = stack
        self.ops = []
        self.n_t = 0
        self.nflush = 0
        self.max_flush = None
        self.finalizing = False
        self._init_sync()

    def sb(self, name, shape, dtype):
        return self.stack.enter_context(self.nc.sbuf_tensor(name, list(shape), dtype))

    def ps(self, name, shape, dtype=F32):
        return self.stack.enter_context(self.nc.psum_tensor(name, list(shape), dtype))

    def dram(self, name, shape, dtype):
        return self.nc.dram_tensor(name, list(shape), dtype)

    def op(self, eng, fn, reads, writes, dma=False):
        if self.max_flush is not None and self.nflush >= self.max_flush and not self.finalizing:
            return
        self.ops.append((eng, fn, tuple(reads), tuple(writes), dma))

    def mm(self, out, lhsT, rhs, start=True, stop=True, rk=None, wk=None):
        self.op('pe', lambda e: e.matmul(out, lhsT, rhs, start=start, stop=stop),
                rk if rk is not None else [K(lhsT), K(rhs)], wk if wk is not None else [K(out)])

    def tr(self, out, in_, ident, rk=None, wk=None):
        self.op('pe', lambda e: e.transpose(out, in_, ident),
                rk if rk is not None else [K(in_), K(ident)], wk if wk is not None else [K(out)])

    def act(self, out, in_, func, bias=None, scale=None, accum_out=None, rk=None, wk=None):
        reads = [K(in_)]
        kw = {}
        if bias is not None:
            kw['bias'] = bias
            if not isinstance(bias, (int, float)):
                reads.append(K(bias))
        if scale is not None:
            kw['scale'] = scale
            if not isinstance(scale, (int, float)):
                reads.append(K(scale))
        writes = [K(out)]
        if accum_out is not None:
            kw['accum_out'] = accum_out
            writes.append(K(accum_out))
        self.op('act', lambda e: e.activation(out, in_, func, **kw),
                rk if rk is not None else reads, wk if wk is not None else writes)

    def ts(self, eng, out, in0, s1, s2, op0, op1=None, rk=None, wk=None):
        reads = [K(in0)]
        for s in (s1, s2):
            if s is not None and not isinstance(s, (int, float)):
                reads.append(K(s))
        if op1 is None:
            f = lambda e: e.tensor_scalar(out, in0, s1, None, op0)
        else:
            f = lambda e: e.tensor_scalar(out, in0, s1, s2, op0, op1)
        self.op(eng, f, rk if rk is not None else reads, wk if wk is not None else [K(out)])

    def tt(self, eng, out, in0, in1, op, rk=None, wk=None):
        self.op(eng, lambda e: e.tensor_tensor(out, in0, in1, op),
                rk if rk is not None else [K(in0), K(in1)], wk if wk is not None else [K(out)])

    def stt(self, out, in0, scalar, in1, op0, op1, rk=None, wk=None):
        reads = [K(in0), K(in1)]
        if not isinstance(scalar, (int, float)):
            reads.append(K(scalar))
        self.op('dve', lambda e: e.scalar_tensor_tensor(out, in0, scalar, in1, op0, op1),
                rk if rk is not None else reads, wk if wk is not None else [K(out)])

    def copy(self, eng, out, in_, rk=None, wk=None):
        if eng == 'act':
            f = lambda e: e.activation(out, in_, AF.Copy)
        else:
            f = lambda e: e.tensor_copy(out, in_)
        self.op(eng, f, rk if rk is not None else [K(in_)], wk if wk is not None else [K(out)])

    def memset(self, eng, out, val, wk=None):
        self.op(eng, lambda e: e.memset(out, val), [], wk if wk is not None else [K(out)])

    def dma(self, eng, out, in_, rk=None, wk=None, **kw):
        self.op(eng, lambda e: e.dma_start(out=out, in_=in_, **kw),
                rk if rk is not None else [K(in_)], wk if wk is not None else [K(out)], dma=True)

    def _init_sync(self):
        nc = self.nc
        self.names = ['pe', 'dve', 'act', 'pool', 'sp']
        self.NDS = 24
        self.semobj = {e: self.root.enter_context(nc.semaphore("c_" + e)) for e in self.names}
        for i in range(self.NDS):
            self.semobj[i] = self.root.enter_context(nc.semaphore("d_%d" % i))
        self.ccnt = {e: 0 for e in self.names}
        self.dcnt = [0] * self.NDS
        self.ndma = 0
        self.waited = {e: {} for e in self.names}

    def flush(self, serialize=False, final=False):
        if self.max_flush is not None and self.nflush >= self.max_flush and not final:
            self.ops = []
            return
        self._flush(serialize, final)
        self.nflush += 1
        if self.max_flush is not None and self.nflush >= self.max_flush:
            self.ops = []

    def _flush(self, serialize=False, final=False):
        nc = self.nc
        names, NDS = self.names, self.NDS
        ccnt, dcnt, waited, semobj = self.ccnt, self.dcnt, self.waited, self.semobj
        barrier = []
        for s in range(NDS):
            if dcnt[s] > 0:
                barrier.append((s, 16 * dcnt[s]))
        for e in names:
            if ccnt[e] > 0:
                barrier.append((e, ccnt[e]))
        last_w = {}
        readers = {}
        tok = []
        streams = {e: [] for e in names}
        first = {e: True for e in names}
        for i, (eng, fn, reads, writes, dma) in enumerate(self.ops):
            deps = set()
            for k in reads:
                if k in last_w:
                    deps.add(last_w[k])
            for k in writes:
                if k in last_w:
                    deps.add(last_w[k])
                for r in readers.get(k, ()):
                    deps.add(r)
            if serialize and i > 0:
                deps.add(i - 1)
            waits = {}
            if first[eng]:
                first[eng] = False
                for sk, val in barrier:
                    waits[sk] = val
            for j in deps:
                sk, val, jeng, jdma = tok[j]
                if jeng == 'pe' and eng == 'pe' and not jdma:
                    continue
                if waits.get(sk, 0) < val:
                    waits[sk] = val
            if dma:
                slot = self.ndma % NDS
                self.ndma += 1
                if dcnt[slot] > 0:
                    v = 16 * dcnt[slot]
                    if waits.get(slot, 0) < v:
                        waits[slot] = v
                dcnt[slot] += 1
                t = (slot, 16 * dcnt[slot], eng, True)
            else:
                ccnt[eng] += 1
                t = (eng, ccnt[eng], eng, False)
            tok.append(t)
            wl = []
            for sk, val in waits.items():
                if waited[eng].get(sk, 0) < val:
                    waited[eng][sk] = val
                    wl.append((sk, val))
            streams[eng].append((wl, fn, t))
            for k in reads:
                readers.setdefault(k, []).append(i)
            for k in writes:
                last_w[k] = i
                readers[k] = []
        fin = []
        if final:
            for s in range(NDS):
                if dcnt[s] > 0:
                    fin.append((s, 16 * dcnt[s]))
            for e in names:
                if ccnt[e] > 0:
                    fin.append((e, ccnt[e]))
        self.n_t += len(self.ops)
        self.ops = []

        def run(e, name):
            for wl, fn, t in streams[name]:
                for sk, val in wl:
                    e.wait_ge(semobj[sk], val)
                ins = fn(e)
                ins.then_inc(semobj[t[0]], 16 if t[3] else 1)
            if name == 'sp':
                for sk, val in fin:
                    e.wait_ge(semobj[sk], val)

        with nc.Block() as block:
            @block.tensor
            def _(e):
                run(e, 'pe')

            @block.vector
            def _(e):
                run(e, 'dve')

            @block.scalar
            def _(e):
                run(e, 'act')

            @block.gpsimd
            def _(e):
                run(e, 'pool')

            @block.sync
            def _(e):
                run(e, 'sp')


class Ctx:
    pass


def setup_common(P, C):
    C.ps = [P.ps("psb%d" % i, [128, 512]) for i in range(8)]
    C.psi = 0
    C.ident = P.sb("ident", [128, 128], F32)
    C.identb = P.sb("identb", [128, 128], BF16)
    C.iota_p = P.sb("iota_p", [128, 1], F32)
    C.iota_f = P.sb("iota_f", [128, 2048], F32)
    C.ones_b = P.sb("ones_b", [128, 128], BF16)
    P.op('pool', lambda e: e.iota(C.iota_p[:], [[0, 1]], base=0, channel_multiplier=1,
                                  allow_small_or_imprecise_dtypes=True), [], [K(C.iota_p[:])])
    P.op('pool', lambda e: e.iota(C.iota_f[:], [[1, 2048]], base=0, channel_multiplier=0,
                                  allow_small_or_imprecise_dtypes=True), [], [K(C.iota_f[:])])
    P.ts('dve', C.ident[:], C.iota_f[:, 0:128], C.iota_p[:, 0:1], None, ALU.is_equal)
    P.copy('dve', C.identb[:], C.ident[:])
    P.memset('dve', C.ones_b[:], 1.0)
    C.halfpi = P.sb("halfpi", [128, 1], F32)
    P.memset('dve', C.halfpi[:], math.pi / 2.0)


def next_ps(C, n=8):
    p = C.ps[C.psi % n]
    C.psi += 1
    return p


def load_bcast(P, name, src_row, n, eng='sp'):
    t = P.sb(name, [128, n], F32)
    P.dma(eng, t[:], src_row.broadcast_to([128, n]))
    return t


def transpose_tile_to_T(P, C, src_tile, dstT, tcol, dst32=None):
    for kb in range(8):
        ps = next_ps(C)
        for j in range(4):
            k = kb * 4 + j
            P.tr(ps[:, j * 128:(j + 1) * 128], src_tile[:, k * 128:(k + 1) * 128], C.ident[:])
        P.copy(('act' if kb % 2 else 'dve') if dst32 is None else 'dve',
               dstT[:, kb * 4:(kb + 1) * 4, tcol:tcol + 128],
               ps[:].rearrange("p (j t) -> p j t", j=4))
        if dst32 is not None:
            P.copy('act',
                   dst32[:, kb * 4:(kb + 1) * 4, :],
                   ps[:].rearrange("p (j t) -> p j t", j=4))


def ln_tile(P, C, r, stats, mv, gt, bt, out, width=D, eps=LN_EPS, tmpname=None):
    nch = width // 512
    for j in range(nch):
        P.op('dve', (lambda j: (lambda e: e.bn_stats(stats[:, j, :], r[:, j * 512:(j + 1) * 512])))(j),
             [K(r)], [K(stats[:])])
    P.op('dve', lambda e: e.bn_aggr(mv[:, 0:2], stats[:, 0:nch, :]), [K(stats[:])], [K(mv[:])])
    P.ts('dve', mv[:, 2:3], mv[:, 1:2], eps, None, ALU.add)
    P.act(mv[:, 3:4], mv[:, 2:3], AF.Sqrt)
    P.op('dve', lambda e: e.reciprocal(mv[:, 4:5], mv[:, 3:4]), [K(mv[:])], [K(mv[:])])
    P.ts('dve', out, r, mv[:, 0:1], mv[:, 4:5], ALU.subtract, ALU.mult)
    P.tt('pool', out, out, gt[:, 0:width], ALU.mult)
    P.tt('pool', out, out, bt[:, 0:width], ALU.add)


def phase_moe(P, C, W, L, rpre_d, out_d, Y_d, tag):
    root = P.stack
    with ExitStack() as st0:
        P.stack = st0
        hT = P.sb(tag + "hT", [128, 32, NT], BF16)
        G = P.sb(tag + "G", [128, NTILE, NEXP], F32)
        with ExitStack() as st:
            P.stack = st
            g1 = load_bcast(P, tag + "g1", W['ln_g'][L, 0:1, :], D)
            b1 = load_bcast(P, tag + "b1", W['ln_b'][L, 0:1, :], D)
            brt = load_bcast(P, tag + "brt", W['moe_b_router'][L:L + 1, :], NEXP)
            wr = P.sb(tag + "wr", [128, 32, NEXP], F32)
            P.dma('sp', wr[:], W['moe_w_router'][L].rearrange("(k p) e -> p k e", p=128))
            rt = [P.sb(tag + "rt%d" % i, [128, D], F32) for i in range(2)]
            h32 = P.sb(tag + "h32", [128, 32, 128], F32)
            stats = P.sb(tag + "stats", [128, 8, 6], F32)
            mv = P.sb(tag + "mv", [128, 8], F32)
            lg = P.sb(tag + "lg", [128, NEXP], F32)
            top8 = P.sb(tag + "top8", [128, 8], F32)
            sm = P.sb(tag + "sm", [128, 8], F32)
            ya = P.sb(tag + "ya", [128, D], F32)
            for i in range(NTILE):
                r = rt[i % 2]
                P.dma('sp', r[:], rpre_d[i * 128:(i + 1) * 128, :])
                ln_tile(P, C, r[:], stats, mv, g1, b1, r[:])
                P.ts('dve', ya[:], r[:], ALPHA, None, ALU.mult)
                P.dma('sp', Y_d[i * 128:(i + 1) * 128, :], ya[:])
                transpose_tile_to_T(P, C, r, hT, i * 128, dst32=h32)
                ps = next_ps(C)
                for k in range(32):
                    P.mm(ps[:, 0:NEXP], h32[:, k, :], wr[:, k, :], start=(k == 0), stop=(k == 31))
                P.tt('dve', lg[:], ps[:, 0:NEXP], brt[:], ALU.add)
                P.op('dve', lambda e: e.max(top8[:], lg[:]), [K(lg[:])], [K(top8[:])])
                P.ts('dve', sm[:, 0:1], top8[:, 0:1], -1.0, None, ALU.mult)
                P.act(G[:, i, :], lg[:], AF.Exp, bias=sm[:, 0:1], scale=1.0)
                P.stt(G[:, i, :], lg[:], top8[:, 3:4], G[:, i, :], ALU.is_ge, ALU.mult)
                P.op('dve', (lambda i: (lambda e: e.tensor_reduce(sm[:, 1:2], G[:, i, :], AX.X, ALU.add)))(i),
                     [K(G[:])], [K(sm[:])])
                P.op('dve', lambda e: e.reciprocal(sm[:, 2:3], sm[:, 1:2]), [K(sm[:])], [K(sm[:])])
                P.ts('dve', G[:, i, :], G[:, i, :], sm[:, 2:3], None, ALU.mult)
            P.flush()
        with ExitStack() as st:
            P.stack = st
            wgu = [P.sb(tag + "wgu%d" % i, [128, 32, 256], BF16) for i in range(2)]
            wdn = [P.sb(tag + "wdn%d" % i, [128, 8, 512], BF16) for i in range(2)]
            bgu = [P.sb(tag + "bgu%d" % i, [128, 16], F32) for i in range(2)]
            bdn = [P.sb(tag + "bdn%d" % i, [1, D], BF16) for i in range(2)]
            gl = [P.sb(tag + "gl%d" % i, [128, 2, NT], F32) for i in range(2)]
            tmp = [P.sb(tag + "tmp%d" % i, [128, NT], F32) for i in range(2)]
            actT = P.sb(tag + "actT", [128, 8, NT], BF16)
            yo = [P.sb(tag + "yo%d" % i, [128, 512], F32) for i in range(4)]
            nw = nd = ny = 0
            for e_ in range(NEXP):
                bg = bgu[e_ % 2]
                P.dma('sp', bg[:], W['moe_b_gu_T'][L, e_])
                bd = bdn[e_ % 2]
                P.dma('pool', bd[:], W['moe_b_down'][L, e_:e_ + 1, :], max_dma_last_dim=8192)
                wsrc = W['moe_w_gu'][L, e_].rearrange("(k p) n -> p k n", p=128)
                for pr in range(4):
                    gb = gl[pr % 2]
                    for part in range(2):
                        wb = wgu[nw % 2]
                        nw += 1
                        c0 = (0 if part == 0 else 8) + 2 * pr
                        P.dma('pool', wb[:], wsrc[:, :, c0 * 128:(c0 + 2) * 128])
                        for sc in range(2):
                            cc = c0 + sc
                            for tb in range(2):
                                ps = next_ps(C)
                                for k in range(32):
                                    P.mm(ps[:], wb[:, k, sc * 128:(sc + 1) * 128],
                                         hT[:, k, tb * 512:(tb + 1) * 512], start=(k == 0), stop=(k == 31))
                                sl = slice(tb * 512, (tb + 1) * 512)
                                if part == 0:
                                    t_ = tmp[0]
                                    P.ts('dve', gb[:, sc, sl], ps[:], bg[:, cc:cc + 1], 7.0, ALU.add, ALU.min)
                                    P.act(t_[:, sl], gb[:, sc, sl], AF.Sigmoid, scale=1.702)
                                    P.tt('pool', gb[:, sc, sl], gb[:, sc, sl], t_[:, sl], ALU.mult)
                                else:
                                    t_ = tmp[1]
                                    P.ts('dve', t_[:, sl], ps[:], bg[:, cc:cc + 1], 7.0, ALU.add, ALU.min)
                                    P.ts('dve', t_[:, sl], t_[:, sl], -7.0, 1.0, ALU.max, ALU.add)
                                    P.tt('pool', actT[:, 2 * pr + sc, sl], t_[:, sl], gb[:, sc, sl], ALU.mult)
                dsrc = W['moe_w_down'][L, e_].rearrange("(k p) n -> p k n", p=128)
                for cb in range(8):
                    wd = wdn[nd % 2]
                    nd += 1
                    P.dma('pool', wd[:], dsrc[:, :, cb * 512:(cb + 1) * 512])
                    for i in range(NTILE):
                        ps = next_ps(C)
                        for k in range(8):
                            P.mm(ps[:], actT[:, k, i * 128:(i + 1) * 128], wd[:, k, :], start=(k == 0), stop=False)
                        P.mm(ps[:], C.ones_b[0:1, :], bd[0:1, cb * 512:(cb + 1) * 512], start=False, stop=True)
                        y = yo[ny % 4]
                        ny += 1
                        P.ts('dve', y[:], ps[:], G[:, i, e_:e_ + 1], None, ALU.mult)
                        P.dma('pool', Y_d[i * 128:(i + 1) * 128, cb * 512:(cb + 1) * 512], y[:],
                              accum_op=ALU.add, rk=[K(y[:]), ('Y', i, cb)], wk=[('Y', i, cb)])
            P.flush()
    with ExitStack() as st:
        P.stack = st
        g2 = load_bcast(P, tag + "g2", W['ln_g'][L, 1:2, :], D)
        b2 = load_bcast(P, tag + "b2", W['ln_b'][L, 1:2, :], D)
        rt = [P.sb(tag + "frt%d" % i, [128, D], F32) for i in range(2)]
        stats = P.sb(tag + "fstats", [128, 8, 6], F32)
        mv = P.sb(tag + "fmv", [128, 8], F32)
        for i in range(NTILE):
            r = rt[i % 2]
            P.dma('sp', r[:], Y_d[i * 128:(i + 1) * 128, :])
            ln_tile(P, C, r[:], stats, mv, g2, b2, r[:])
            P.dma('sp', out_d[i * 128:(i + 1) * 128, :], r[:])
        P.flush()
    P.stack = root
def gelu_from(P, C, src, out, t1, t2, n):
    P.act(t1[:, 0:n], src, AF.Square)
    P.ts('dve', t1[:, 0:n], t1[:, 0:n], 0.044715, 1.0, ALU.mult, ALU.add)
    P.tt('dve', t1[:, 0:n], t1[:, 0:n], src, ALU.mult)
    P.act(t2[:, 0:n], t1[:, 0:n], AF.Sigmoid, scale=1.5957691216057308)
    P.tt('dve', out, t2[:, 0:n], src, ALU.mult)


def sincos(P, C, ang, o_sin, o_cos, tmp):
    P.ts('dve', tmp, ang, 1.0 / TWO_PI, MAGIC, ALU.mult, ALU.add)
    P.ts('dve', tmp, tmp, -MAGIC, None, ALU.add)
    P.stt(o_cos, tmp, -CW1, ang, ALU.mult, ALU.add)
    P.stt(o_cos, tmp, -CW2, o_cos, ALU.mult, ALU.add)
    P.ts('dve', o_cos, o_cos, -math.pi, math.pi, ALU.max, ALU.min)
    P.act(o_sin, o_cos, AF.Sin)
    P.act(o_cos, o_cos, AF.Abs)
    P.act(o_cos, o_cos, AF.Sin, scale=-1.0, bias=C.halfpi[:, 0:1])


def build_xT(P, C, x_d, xT, xt2):
    for i in range(NTILE):
        xt = xt2[i % 2]
        P.dma('sp', xt[:], x_d[i * 128:(i + 1) * 128, :])
        transpose_tile_to_T(P, C, xt, xT, i * 128)


def inproj_ssm(P, C, W, xT, wbuf, uo, uT_d, coff):
    wsrc = W['hy_w_in'].rearrange("(k p) n -> p k n", p=128)
    n = 0
    for c2 in range(8):
        wb = wbuf[c2 % 2]
        P.dma('pool', wb[:], wsrc[:, :, 4096 + c2 * 256:4096 + (c2 + 1) * 256])
        for sc in range(2):
            c = 2 * c2 + sc
            for tb in range(2):
                ps = next_ps(C)
                for k in range(32):
                    P.mm(ps[:], wb[:, k, sc * 128:(sc + 1) * 128], xT[:, k, tb * 512:(tb + 1) * 512],
                         start=(k == 0), stop=(k == 31))
                u = uo[n % 2]
                n += 1
                P.copy('act', u[:], ps[:])
                P.dma('sp', uT_d[c * 128:(c + 1) * 128, coff + tb * 512:coff + (tb + 1) * 512], u[:])


def phase_l0(P, C, W, x_d, xp_d, rpre_d, S):
    root = P.stack
    uT_d, catT_d = S['uT_d'], S['catT_d']
    with ExitStack() as st0:
        P.stack = st0
        xT = P.sb("xT", [128, 32, NT], BF16)
        xt2 = [P.sb("xt%d" % i, [128, D], F32) for i in range(2)]
        wbuf = [P.sb("wb%d" % i, [128, 32, 256], BF16) for i in range(2)]
        uo = [P.sb("uo%d" % i, [128, 512], BF16) for i in range(2)]
        build_xT(P, C, xp_d, xT, xt2)
        inproj_ssm(P, C, W, xT, wbuf, uo, uT_d, 0)
        P.flush()
        build_xT(P, C, x_d, xT, xt2)
        inproj_ssm(P, C, W, xT, wbuf, uo, uT_d, NT)
        P.flush()
        with ExitStack() as st:
            P.stack = st
            wsT = P.sb("wsT", [128, 8, 128], BF16)
            wsl = P.sb("wsl", [128, 8, 128], F32)
            bsr = P.sb("bsr", [1, 8, 128], BF16)
            gmg = P.sb("gmg", [128, 8, 256], F32)
            gmb = P.sb("gmb", [128, 8, 256], F32)
            ugT = P.sb("ugT", [128, 2, NT], BF16)
            ygT = P.sb("ygT", [128, 2, NT], BF16)
            v32 = P.sb("v32", [128, 256], F32)
            vn = P.sb("vn", [128, 256], BF16)
            t1 = P.sb("gt1", [128, 512], F32)
            t2 = P.sb("gt2", [128, 512], F32)
            st6 = P.sb("gst", [128, 1, 6], F32)
            mv = P.sb("gmv", [128, 8], F32)
            P.dma('sp', wsl[:], W['gm_w_s'].rearrange("g i j -> i g j"))
            P.dma('pool', bsr[:], W['gm_b_s'].rearrange("(o g) i -> o g i", o=1))
            P.dma('sp', gmg[:], W['gm_ln_g'].rearrange("(o g) c -> o g c", o=1).broadcast_to([128, 8, 256]))
            P.dma('sp', gmb[:], W['gm_ln_b'].rearrange("(o g) c -> o g c", o=1).broadcast_to([128, 8, 256]))
            for g in range(8):
                ps = next_ps(C)
                P.tr(ps[:, 0:128], wsl[:, g, :], C.ident[:])
                P.copy('dve', wsT[:, g, :], ps[:, 0:128])
            P.memset('dve', wsT[64:128, :, 0:64], 0.0)
            wsrc = W['hy_w_in'].rearrange("(k p) n -> p k n", p=128)
            nb = 0
            for g in range(8):
                wb = wbuf[nb % 2]
                nb += 1
                P.dma('pool', wb[:], wsrc[:, :, g * 256:(g + 1) * 256])
                for sc in range(2):
                    for tb in range(2):
                        ps = next_ps(C)
                        for k in range(32):
                            P.mm(ps[:], wb[:, k, sc * 128:(sc + 1) * 128], xT[:, k, tb * 512:(tb + 1) * 512],
                                 start=(k == 0), stop=(k == 31))
                        gelu_from(P, C, ps[:], ugT[:, sc, tb * 512:(tb + 1) * 512], t1, t2, 512)
                wb2 = wbuf[nb % 2]
                nb += 1
                P.dma('pool', wb2[:], wsrc[:, :, 2048 + g * 256:2048 + (g + 1) * 256])
                for i in range(NTILE):
                    ps = next_ps(C)
                    for k in range(32):
                        P.mm(ps[:, 0:256], xT[:, k, i * 128:(i + 1) * 128], wb2[:, k, :],
                             start=(k == 0), stop=(k == 31))
                    gelu_from(P, C, ps[:, 0:256], v32[:], t1, t2, 256)
                    P.op('dve', lambda e: e.bn_stats(st6[:, 0, :], v32[:]), [K(v32[:])], [K(st6[:])])
                    P.op('dve', lambda e: e.bn_aggr(mv[:, 0:2], st6[:]), [K(st6[:])], [K(mv[:])])
                    P.ts('dve', mv[:, 2:3], mv[:, 1:2], LN_EPS, None, ALU.add)
                    P.act(mv[:, 3:4], mv[:, 2:3], AF.Sqrt)
                    P.op('dve', lambda e: e.reciprocal(mv[:, 4:5], mv[:, 3:4]), [K(mv[:])], [K(mv[:])])
                    P.ts('dve', v32[:], v32[:], mv[:, 0:1], mv[:, 4:5], ALU.subtract, ALU.mult)
                    P.tt('pool', v32[:], v32[:], gmg[:, g, :], ALU.mult)
                    P.tt('pool', vn[:], v32[:], gmb[:, g, :], ALU.add)
                    for sc in range(2):
                        ps2 = next_ps(C)
                        P.mm(ps2[:, 0:128], vn[:, sc * 128:(sc + 1) * 128], wsT[:, g, :], start=True, stop=False)
                        P.mm(ps2[:, 0:128], C.ones_b[0:1, :], bsr[0:1, g, :], start=False, stop=True)
                        P.tt('dve', ygT[:, sc, i * 128:(i + 1) * 128], ps2[:, 0:128],
                             ugT[:, sc, i * 128:(i + 1) * 128], ALU.mult)
                for sc in range(2):
                    P.dma('sp', catT_d[g * 256 + sc * 128:g * 256 + (sc + 1) * 128, :], ygT[:, sc, :])
            P.flush()
    with ExitStack() as st:
        P.stack = st
        ysT = P.sb("ysT", [128, 16, NT], BF16)
        Bl = P.sb("Bl", [128, 16, 2, 128], BF16)
        Cl = P.sb("Cl", [128, 64, 2, 32], BF16)
        rS = P.sb("rS", [128, 64], F32)
        thS = P.sb("thS", [128, 64], F32)
        c512 = P.sb("c512", [128, 64], F32)
        s512 = P.sb("s512", [128, 64], F32)
        dT = P.sb("dT", [128, 16], F32)
        Bl3 = P.sb("Bl3", [128, 16, 2, 128], BF16)
        Cl3 = P.sb("Cl3", [128, 16, 2, 64], BF16)
        with ExitStack() as st2:
            P.stack = st2
            shp = [128, 16, 128]
            lr = P.sb("lr", shp, F32)
            li = P.sb("li", shp, F32)
            dtb = P.sb("dtb", shp, F32)
            ta = P.sb("ta", shp, F32)
            tb_ = P.sb("tb_", shp, F32)
            tc = P.sb("tc", shp, F32)
            td = P.sb("td", shp, F32)
            te = P.sb("te", shp, F32)
            bre = P.sb("bre", shp, F32)
            bim = P.sb("bim", shp, F32)
            cl32 = P.sb("cl32", [128, 64, 2, 32], F32)
            s64 = [P.sb("s64_%d" % i, [128, 64], F32) for i in range(4)]
            P.dma('sp', lr[:], W['lamre_B'])
            P.dma('sp', li[:], W['lamim_B'])
            P.dma('sp', dtb[:], W['logdt_B'])
            P.dma('sp', bre[:], W['bre_B'])
            P.dma('sp', bim[:], W['bim_B'])
            P.dma('sp', cl32[:], W['cl_S'])
            P.dma('sp', dT[:], W['d_T'])
            P.dma('sp', s64[0][:], W['lamre_S'])
            P.dma('sp', s64[1][:], W['lamim_S'])
            P.dma('sp', s64[2][:], W['logdt_S'])
            P.act(dtb[:], dtb[:], AF.Exp)
            P.tt('dve', ta[:], lr[:], dtb[:], ALU.mult)
            P.act(ta[:], ta[:], AF.Exp)
            P.tt('dve', tb_[:], li[:], dtb[:], ALU.mult)
            sincos(P, C, tb_[:], tc[:], td[:], te[:])
            P.tt('dve', tc[:], tc[:], ta[:], ALU.mult)
            P.tt('dve', td[:], td[:], ta[:], ALU.mult)
            P.ts('dve', td[:], td[:], -1.0, None, ALU.add)
            P.tt('dve', ta[:], lr[:], lr[:], ALU.mult)
            P.tt('dve', te[:], li[:], li[:], ALU.mult)
            P.tt('dve', ta[:], ta[:], te[:], ALU.add)
            P.op('dve', lambda e: e.reciprocal(ta[:], ta[:]), [K(ta[:])], [K(ta[:])])
            P.tt('dve', te[:], td[:], lr[:], ALU.mult)
            P.tt('dve', tb_[:], tc[:], li[:], ALU.mult)
            P.tt('dve', te[:], te[:], tb_[:], ALU.add)
            P.tt('dve', te[:], te[:], ta[:], ALU.mult)
            P.tt('dve', tb_[:], tc[:], lr[:], ALU.mult)
            P.tt('dve', td[:], td[:], li[:], ALU.mult)
            P.tt('dve', tb_[:], tb_[:], td[:], ALU.subtract)
            P.tt('dve', tb_[:], tb_[:], ta[:], ALU.mult)
            P.tt('dve', tc[:], te[:], bre[:], ALU.mult)
            P.tt('dve', td[:], tb_[:], bim[:], ALU.mult)
            P.tt('dve', Bl[:, :, 0, :], tc[:], td[:], ALU.subtract)
            P.tt('dve', tc[:], te[:], bim[:], ALU.mult)
            P.tt('dve', td[:], tb_[:], bre[:], ALU.mult)
            P.tt('dve', Bl[:, :, 1, :], tc[:], td[:], ALU.add)
            P.copy('dve', Cl[:, :, 0, :], cl32[:, :, 0, :])
            P.ts('dve', Cl[:, :, 1, :], cl32[:, :, 1, :], -1.0, None, ALU.mult)
            P.copy('dve', Bl3[64:128, :, :, :], Bl[64:128, :, :, :])
            P.memset('dve', Bl3[64:96, :, :, :], 0.0)
            P.memset('dve', Cl3[:], 0.0)
            for c_ in range(16):
                P.copy('dve', Cl3[:, c_, :, 32:64], Cl[:, 4 * c_ + 3, :, :])
            P.act(s64[2][:], s64[2][:], AF.Exp)
            P.tt('dve', rS[:], s64[0][:], s64[2][:], ALU.mult)
            P.act(rS[:], rS[:], AF.Exp)
            P.tt('dve', thS[:], s64[1][:], s64[2][:], ALU.mult)
            P.ts('dve', s64[0][:], thS[:], 512.0, None, ALU.mult)
            sincos(P, C, s64[0][:], s512[:], c512[:], s64[3][:])
            P.flush()
        P.stack = st
        uTc = [P.sb("uTc%d" % i, [128, 2 * NT], BF16) for i in range(2)]
        ang = P.sb("ang", [128, 512], F32)
        tmpa = P.sb("tmpa", [128, 512], F32)
        Sn = [P.sb("Sn%d" % i, [128, 512], F32) for i in range(2)]
        Cc = [P.sb("Cc%d" % i, [128, 512], F32) for i in range(2)]
        rb = [P.sb("rb%d" % i, [128, 512], F32) for i in range(2)]
        tq = [P.sb("tq%d" % i, [128, 512], F32) for i in range(4)]
        xr = [P.sb("xr%d" % i, [128, 512], F32) for i in range(2)]
        xi = [P.sb("xi%d" % i, [128, 512], F32) for i in range(2)]
        gr = [P.sb("gr%d" % i, [128, 512], F32) for i in range(2)]
        gi = [P.sb("gi%d" % i, [128, 512], F32) for i in range(2)]
        dq = [P.sb("dq%d" % i, [128, 512], F32) for i in range(4)]
        hre = [P.sb("hre%d" % i, [128, 512], BF16) for i in range(2)]
        him = [P.sb("him%d" % i, [128, 512], BF16) for i in range(2)]
        ini = [P.sb("ini%d" % i, [128, 4], F32) for i in range(2)]
        yv = P.sb("yv", [128, 512], F32)
        g1 = P.sb("sg1", [128, 512], F32)
        g2 = P.sb("sg2", [128, 512], F32)
        nblk = 0
        for c in range(16):
            uc = uTc[c % 2]
            P.dma('sp', uc[:], uT_d[c * 128:(c + 1) * 128, :])
            psY = [C.ps[6], C.ps[7]]
            for q in (0, 1, 3, 2):
                j = 4 * c + q
                sn, cc, rbt = Sn[j % 2], Cc[j % 2], rb[j % 2]
                P.ts('dve', ang[:], C.iota_f[:, 0:512], thS[:, j:j + 1], None, ALU.mult)
                sincos(P, C, ang[:], sn[:], cc[:], tmpa[:])
                P.ts('dve', rbt[:], C.iota_f[:, 0:512], 0.0, rS[:, j:j + 1], ALU.mult, ALU.add)
                for tb in range(4):
                    nblk += 1
                    b2 = nblk % 2
                    ps_re = next_ps(C, 6)
                    ps_im = next_ps(C, 6)
                    if q < 3:
                        usl = uc[32 * q:32 * q + 32, tb * 512:(tb + 1) * 512]
                        P.mm(ps_re[:], Bl[32 * q:32 * q + 32, c, 0, :], usl)
                        P.mm(ps_im[:], Bl[32 * q:32 * q + 32, c, 1, :], usl)
                    else:
                        usl = uc[64:128, tb * 512:(tb + 1) * 512]
                        P.mm(ps_re[:], Bl3[64:128, c, 0, :], usl)
                        P.mm(ps_im[:], Bl3[64:128, c, 1, :], usl)
                    P.tt('dve', tq[0][:], ps_re[:], cc[:], ALU.mult)
                    P.tt('dve', tq[1][:], ps_im[:], sn[:], ALU.mult)
                    P.tt('pool', xr[b2][:], tq[0][:], tq[1][:], ALU.add)
                    P.tt('dve', tq[2][:], ps_im[:], cc[:], ALU.mult)
                    P.tt('dve', tq[3][:], ps_re[:], sn[:], ALU.mult)
                    P.tt('pool', xi[b2][:], tq[2][:], tq[3][:], ALU.subtract)
                    if tb == 0:
                        i_r, i_i = 0.0, 0.0
                        rk_extra = []
                    else:
                        i_r, i_i = ini[1 - b2][:, 0:1], ini[1 - b2][:, 1:2]
                        rk_extra = [K(ini[1 - b2][:])]
                    P.op('dve', (lambda o, d0, d1, i0: (lambda e: e.tensor_tensor_scan(o, d0, d1, i0, ALU.mult, ALU.add)))(
                        gr[b2][:], rbt[:], xr[b2][:], i_r), [K(rbt[:]), K(xr[b2][:])] + rk_extra, [K(gr[b2][:])])
                    P.op('dve', (lambda o, d0, d1, i0: (lambda e: e.tensor_tensor_scan(o, d0, d1, i0, ALU.mult, ALU.add)))(
                        gi[b2][:], rbt[:], xi[b2][:], i_i), [K(rbt[:]), K(xi[b2][:])] + rk_extra, [K(gi[b2][:])])
                    if tb < 3:
                        it = ini[b2]
                        P.ts('dve', it[:, 2:3], gi[b2][:, 511:512], s512[:, j:j + 1], None, ALU.mult)
                        P.stt(it[:, 0:1], gr[b2][:, 511:512], c512[:, j:j + 1], it[:, 2:3], ALU.mult, ALU.subtract)
                        P.ts('dve', it[:, 3:4], gr[b2][:, 511:512], s512[:, j:j + 1], None, ALU.mult)
                        P.stt(it[:, 1:2], gi[b2][:, 511:512], c512[:, j:j + 1], it[:, 3:4], ALU.mult, ALU.add)
                    if tb >= 2:
                        P.tt('pool', dq[0][:], gr[b2][:], cc[:], ALU.mult)
                        P.tt('pool', dq[1][:], gi[b2][:], sn[:], ALU.mult)
                        P.tt('pool', hre[b2][:], dq[0][:], dq[1][:], ALU.subtract)
                        P.tt('pool', dq[2][:], gi[b2][:], cc[:], ALU.mult)
                        P.tt('pool', dq[3][:], gr[b2][:], sn[:], ALU.mult)
                        P.tt('pool', him[b2][:], dq[2][:], dq[3][:], ALU.add)
                        py = psY[tb - 2]
                        if q == 3:
                            P.mm(py[64:128, :], Cl3[:, c, 0, :], hre[b2][:], start=True, stop=False)
                            P.mm(py[64:128, :], Cl3[:, c, 1, :], him[b2][:], start=False, stop=False)
                        elif q == 2:
                            P.mm(py[64:96, :], Cl[:, j, 0, :], hre[b2][:], start=False, stop=False)
                            P.mm(py[64:96, :], Cl[:, j, 1, :], him[b2][:], start=False, stop=True)
                        else:
                            P.mm(py[32 * q:32 * q + 32, :], Cl[:, j, 0, :], hre[b2][:], start=True, stop=False)
                            P.mm(py[32 * q:32 * q + 32, :], Cl[:, j, 1, :], him[b2][:], start=False, stop=True)
            for tbm in range(2):
                sl = slice(tbm * 512, (tbm + 1) * 512)
                P.stt(yv[:], uc[:, NT + tbm * 512:NT + (tbm + 1) * 512], dT[:, c:c + 1], psY[tbm][:],
                      ALU.mult, ALU.add)
                gelu_from(P, C, yv[:], ysT[:, c, sl], g1, g2, 512)
        P.flush()
        with ExitStack() as st3:
            P.stack = st3
            wgl = [P.sb("wgl%d" % i, [128, 16, 256], BF16) for i in range(2)]
            bgl = P.sb("bgl", [128, 16], F32)
            sg = [P.sb("sgl%d" % i, [128, 512], F32) for i in range(2)]
            yo = [P.sb("gyo%d" % i, [128, 512], BF16) for i in range(2)]
            P.dma('sp', bgl[:], W['bglu_T'])
            gsrc = W['ssm_w_glu'].rearrange("(k p) n -> p k n", p=128)
            n = 0
            for n2 in range(8):
                wb = wgl[n2 % 2]
                P.dma('pool', wb[:], gsrc[:, :, n2 * 256:(n2 + 1) * 256])
                for sc in range(2):
                    nn = 2 * n2 + sc
                    for tb in range(2):
                        sl = slice(tb * 512, (tb + 1) * 512)
                        ps = next_ps(C)
                        for k in range(16):
                            P.mm(ps[:], wb[:, k, sc * 128:(sc + 1) * 128], ysT[:, k, sl], start=(k == 0), stop=(k == 15))
                        s_ = sg[n % 2]
                        y_ = yo[n % 2]
                        n += 1
                        P.act(s_[:], ps[:], AF.Sigmoid, bias=bgl[:, nn:nn + 1], scale=1.0)
                        P.tt('dve', y_[:], s_[:], ysT[:, nn, sl], ALU.mult)
                        P.dma('sp', catT_d[2048 + nn * 128:2048 + (nn + 1) * 128, sl], y_[:])
            P.flush()
    with ExitStack() as st:
        P.stack = st
        catT = P.sb("catT", [128, 32, NT], BF16)
        wo = [P.sb("wo%d" % i, [128, 32, 512], BF16) for i in range(2)]
        xs = [P.sb("xs%d" % i, [128, 512], F32) for i in range(3)]
        P.dma('sp', catT[:], catT_d.rearrange("(k p) t -> p k t", p=128))
        osrc = W['hy_w_out'].rearrange("(k p) n -> p k n", p=128)
        n = 0
        for cb in range(8):
            wb = wo[cb % 2]
            P.dma('pool', wb[:], osrc[:, :, cb * 512:(cb + 1) * 512])
            for i in range(NTILE):
                x_ = xs[n % 3]
                n += 1
                P.dma('sp', x_[:], x_d[i * 128:(i + 1) * 128, cb * 512:(cb + 1) * 512])
                ps = next_ps(C)
                for k in range(32):
                    P.mm(ps[:], catT[:, k, i * 128:(i + 1) * 128], wb[:, k, :], start=(k == 0), stop=(k == 31))
                P.stt(x_[:], x_[:], ALPHA, ps[:], ALU.mult, ALU.add)
                P.dma('sp', rpre_d[i * 128:(i + 1) * 128, cb * 512:(cb + 1) * 512], x_[:])
        P.flush()
    P.stack = root
SCALE = 192 ** -0.5


def phase_l1(P, C, W, h_d, hp_d, rpre_d, S):
    root = P.stack
    kvT_d, cqT_d = S['kvT_d'], S['cqT_d']
    with ExitStack() as st0:
        P.stack = st0
        invf = P.sb("invf", [128, 32], F32)
        cosT = P.sb("cosT", [128, NTILE, 32], F32)
        sinT = P.sb("sinT", [128, NTILE, 32], F32)
        P.act(invf[:], C.iota_f[:, 0:32], AF.Exp, scale=-2.0 * math.log(10000.0) / 64.0)
        stH = ExitStack()
        P.stack = stH
        hT = P.sb("ahT", [128, 32, NT], BF16)
        with ExitStack() as st:
            P.stack = st
            xt1 = P.sb("axt0", [128, D], F32)
            xt2 = [xt1, xt1]
            wkv = P.sb("awkv", [128, 32, 576], BF16)
            gkv = load_bcast(P, "agkv", W['mla_kv_norm_g'], 512)
            c32 = P.sb("ac32", [128, 1600], F32)
            junk = P.sb("ajunk", [128, 1024], F32)
            ssq = P.sb("assq", [128, 8], F32)
            posi = P.sb("aposi", [128, 1], I32)
            posf = P.sb("aposf", [128, 1], F32)
            ang = P.sb("aang", [128, 32], F32)
            tmpa = P.sb("atmpa", [128, 32], F32)
            cs = P.sb("acs", [128, 32], F32)
            sn = P.sb("asn", [128, 32], F32)
            kr = P.sb("akr", [128, 64], F32)
            rt = [P.sb("art%d" % i, [128, 32], F32) for i in range(2)]
            oT = [P.sb("aoT%d" % i, [128, 4, 128], BF16) for i in range(2)]
            okr = [P.sb("aokr%d" % i, [64, 128], BF16) for i in range(2)]
            isrc = W['mla_w_in'].rearrange("(k p) n -> p k n", p=128)
            P.dma('pool', wkv[:], isrc[:, :, 1024:1600])
            nq = 0
            for pas in range(2):
                src_d, pos_d, coff = (hp_d, W['pos_prev'], 0) if pas == 0 else (h_d, W['pos_mine'], NT)
                build_xT(P, C, src_d, hT, xt2)
                for i in range(NTILE):
                    tsl = slice(i * 128, (i + 1) * 128)
                    ps = next_ps(C)
                    ps2 = next_ps(C)
                    for k in range(32):
                        P.mm(ps[:], hT[:, k, tsl], wkv[:, k, 0:512], start=(k == 0), stop=(k == 31))
                    for k in range(32):
                        P.mm(ps2[:, 0:64], hT[:, k, tsl], wkv[:, k, 512:576], start=(k == 0), stop=(k == 31))
                    P.copy('act', c32[:, 1024:1536], ps[:])
                    P.copy('act', c32[:, 1536:1600], ps2[:, 0:64])
                    P.act(junk[:, 0:512], c32[:, 1024:1536], AF.Square, accum_out=ssq[:, 0:1])
                    P.ts('dve', ssq[:, 1:2], ssq[:, 0:1], 1.0 / 512.0, RMS_EPS, ALU.mult, ALU.add)
                    P.act(ssq[:, 2:3], ssq[:, 1:2], AF.Sqrt)
                    P.op('dve', lambda e: e.reciprocal(ssq[:, 3:4], ssq[:, 2:3]), [K(ssq[:])], [K(ssq[:])])
                    P.ts('dve', c32[:, 1024:1536], c32[:, 1024:1536], ssq[:, 3:4], None, ALU.mult)
                    P.tt('pool', c32[:, 1024:1536], c32[:, 1024:1536], gkv[:], ALU.mult)
                    P.dma('sp', posi[:], pos_d[i * 128:(i + 1) * 128, :])
                    P.copy('dve', posf[:], posi[:])
                    P.ts('dve', ang[:], invf[:], posf[:, 0:1], None, ALU.mult)
                    if pas == 1:
                        s_, c_ = sinT[:, i, :], cosT[:, i, :]
                    else:
                        s_, c_ = sn[:], cs[:]
                    sincos(P, C, ang[:], s_, c_, tmpa[:])
                    x1, x2 = c32[:, 1536:1568], c32[:, 1568:1600]
                    P.tt('dve', rt[0][:], x1, c_, ALU.mult)
                    P.tt('dve', rt[1][:], x2, s_, ALU.mult)
                    P.tt('dve', kr[:, 0:32], rt[0][:], rt[1][:], ALU.subtract)
                    P.tt('dve', rt[0][:], x2, c_, ALU.mult)
                    P.tt('dve', rt[1][:], x1, s_, ALU.mult)
                    P.tt('dve', kr[:, 32:64], rt[0][:], rt[1][:], ALU.add)
                    pt = next_ps(C)
                    for j in range(4):
                        P.tr(pt[:, j * 128:(j + 1) * 128], c32[:, 1024 + j * 128:1024 + (j + 1) * 128], C.ident[:])
                    o_ = oT[i % 2]
                    P.copy('dve', o_[:], pt[:].rearrange("p (j t) -> p j t", j=4))
                    P.dma('sp', kvT_d[0:512, coff + i * 128:coff + (i + 1) * 128].rearrange("(j p) t -> p j t", p=128),
                          o_[:])
                    pt2 = next_ps(C)
                    P.tr(pt2[0:64, 0:128], kr[:, 0:64], C.ident[:])
                    ok_ = okr[i % 2]
                    P.copy('dve', ok_[:], pt2[0:64, 0:128])
                    P.dma('sp', kvT_d[512:576, coff + i * 128:coff + (i + 1) * 128], ok_[:])
                P.flush()
        with ExitStack() as st:
            P.stack = st
            wq = [P.sb("awq%d" % i, [128, 32, 512], BF16) for i in range(2)]
            gq = load_bcast(P, "agq", W['mla_q_norm_g'], 1024)
            c32 = P.sb("ac32q", [128, 1024], F32)
            junk = P.sb("ajunkq", [128, 1024], F32)
            ssq = P.sb("assqq", [128, 8], F32)
            oT = [P.sb("aoTq%d" % i, [128, 4, 128], BF16) for i in range(2)]
            isrc = W['mla_w_in'].rearrange("(k p) n -> p k n", p=128)
            for hb in range(2):
                P.dma('pool', wq[hb][:], isrc[:, :, hb * 512:(hb + 1) * 512])
            for i in range(NTILE):
                tsl = slice(i * 128, (i + 1) * 128)
                for hb in range(2):
                    pq = next_ps(C)
                    for k in range(32):
                        P.mm(pq[:], hT[:, k, tsl], wq[hb][:, k, :], start=(k == 0), stop=(k == 31))
                    P.copy('act', c32[:, hb * 512:(hb + 1) * 512], pq[:])
                P.act(junk[:], c32[:, 0:1024], AF.Square, accum_out=ssq[:, 4:5])
                P.ts('dve', ssq[:, 5:6], ssq[:, 4:5], 1.0 / 1024.0, RMS_EPS, ALU.mult, ALU.add)
                P.act(ssq[:, 6:7], ssq[:, 5:6], AF.Sqrt)
                P.op('dve', lambda e: e.reciprocal(ssq[:, 7:8], ssq[:, 6:7]), [K(ssq[:])], [K(ssq[:])])
                P.ts('dve', c32[:, 0:1024], c32[:, 0:1024], ssq[:, 7:8], None, ALU.mult)
                P.tt('pool', c32[:, 0:1024], c32[:, 0:1024], gq[:], ALU.mult)
                for jb in range(2):
                    pt = next_ps(C)
                    for j in range(4):
                        kk = jb * 4 + j
                        P.tr(pt[:, j * 128:(j + 1) * 128], c32[:, kk * 128:(kk + 1) * 128], C.ident[:])
                    o_ = oT[jb]
                    P.copy('dve', o_[:], pt[:].rearrange("p (j t) -> p j t", j=4))
                    P.dma('sp', cqT_d[jb * 512:(jb + 1) * 512, i * 128:(i + 1) * 128].rearrange(
                        "(j p) t -> p j t", p=128), o_[:])
            P.flush()
        stH.close()
        P.stack = st0
        with ExitStack() as st:
            P.stack = st
            OT = P.sb("aOT", [128, 32, NT], BF16)
            cqT = P.sb("acqT", [128, 8, NT], BF16)
            ckvT = P.sb("ackvT", [128, 4, 2 * NT], BF16)
            krT = P.sb("akrT", [64, 2 * NT], BF16)
            Va = P.sb("aVa", [128, 16, 132], BF16)
            flg = P.sb("aflg", [128, 1], F32)
            dmask = P.sb("admask", [128, 1], F32)
            wqh = [P.sb("awqh%d" % i, [128, 8, 192], BF16) for i in range(2)]
            wkh = [P.sb("awkh%d" % i, [128, 4, 256], BF16) for i in range(2)]
            kT = P.sb("akT", [128, 2 * NT], BF16)
            q32 = P.sb("aq32", [128, 192], F32)
            rt = [P.sb("aqr%d" % i, [128, 32], F32) for i in range(2)]
            qT = P.sb("aqT", [128, NT], BF16)
            qrT = P.sb("aqrT", [64, NT], BF16)
            Pb = [P.sb("aPb%d" % i, [128, 2 * NT], BF16) for i in range(2)]
            PT = [P.sb("aPT%d" % i, [128, 16, 128], BF16) for i in range(2)]
            mx = P.sb("amx", [128, 8], F32)
            On = P.sb("aOn", [128, 128], F32)
            P.dma('sp', cqT[:], cqT_d.rearrange("(k p) t -> p k t", p=128))
            P.dma('sp', ckvT[:], kvT_d[0:512, :].rearrange("(k p) t -> p k t", p=128))
            P.dma('sp', krT[:], kvT_d[512:576, :])
            P.dma('sp', flg[:], W['flag'])
            P.ts('dve', dmask[:], C.iota_p[:], 64.0, None, ALU.is_ge)
            P.memset('dve', Va[:], 1.0)
            for kt in range(8):
                P.copy('dve', Va[:, kt, 128:129], flg[:])
            qsrc = W['mla_w_uq'].rearrange("(k p) n -> p k n", p=128)
            ksrc = W['mla_w_ukv'].rearrange("(k p) n -> p k n", p=128)
            npb = 0
            for h in range(32):
                wq_, wk_ = wqh[h % 2], wkh[h % 2]
                P.dma('pool', wq_[:], qsrc[:, :, h * 192:(h + 1) * 192])
                P.dma('pool', wk_[:], ksrc[:, :, h * 256:(h + 1) * 256])
                for tb in range(4):
                    ps = next_ps(C)
                    for k in range(4):
                        P.mm(ps[:], wk_[:, k, 0:128], ckvT[:, k, tb * 512:(tb + 1) * 512], start=(k == 0), stop=(k == 3))
                    P.copy('act', kT[:, tb * 512:(tb + 1) * 512], ps[:])
                for kb in range(4):
                    ps = next_ps(C)
                    for j in range(4):
                        kt = kb * 4 + j
                        for k in range(4):
                            P.mm(ps[:, j * 128:(j + 1) * 128], ckvT[:, k, kt * 128:(kt + 1) * 128], wk_[:, k, 128:256],
                                 start=(k == 0), stop=(k == 3))
                    P.copy('act', Va[:, kb * 4:(kb + 1) * 4, 0:128], ps[:].rearrange("p (j t) -> p j t", j=4))
                for i in range(NTILE):
                    tsl = slice(i * 128, (i + 1) * 128)
                    ps = next_ps(C)
                    for k in range(8):
                        P.mm(ps[:, 0:192], cqT[:, k, tsl], wq_[:, k, :], start=(k == 0), stop=(k == 7))
                    P.copy('act', q32[:], ps[:, 0:192])
                    x1, x2 = ps[:, 128:160], ps[:, 160:192]
                    c_, s_ = cosT[:, i, :], sinT[:, i, :]
                    P.tt('dve', rt[0][:], x1, c_, ALU.mult)
                    P.tt('dve', rt[1][:], x2, s_, ALU.mult)
                    P.tt('dve', q32[:, 128:160], rt[0][:], rt[1][:], ALU.subtract, rk=[K(rt[0][:]), K(rt[1][:])])
                    P.tt('dve', rt[0][:], x2, c_, ALU.mult)
                    P.tt('dve', rt[1][:], x1, s_, ALU.mult)
                    P.tt('dve', q32[:, 160:192], rt[0][:], rt[1][:], ALU.add, rk=[K(rt[0][:]), K(rt[1][:])])
                    pt = next_ps(C)
                    P.tr(pt[:, 0:128], q32[:, 0:128], C.ident[:])
                    P.tr(pt[0:64, 128:256], q32[:, 128:192], C.ident[:])
                    P.copy('dve', qT[:, tsl], pt[:, 0:128])
                    P.copy('dve', qrT[:, tsl], pt[0:64, 128:256])
                for i in range(NTILE):
                    tsl = slice(i * 128, (i + 1) * 128)
                    nk = 9 + i
                    nkeys = nk * 128
                    nb = (nkeys + 511) // 512
                    pb, ptt = Pb[npb % 2], PT[npb % 2]
                    npb += 1
                    banks = []
                    for b in range(nb):
                        w = min(512, nkeys - b * 512)
                        ps = next_ps(C)
                        banks.append((ps, w))
                        P.mm(ps[:, 0:w], qT[:, tsl], kT[:, b * 512:b * 512 + w], start=True, stop=False)
                        P.mm(ps[:, 0:w], qrT[:, tsl], krT[:, b * 512:b * 512 + w], start=False, stop=True)
                        P.op('dve', (lambda o, a: (lambda e: e.tensor_reduce(o, a, AX.X, ALU.max)))(
                            mx[:, b:b + 1], ps[:, 0:w]), [K(ps[:])], [K(mx[:])])
                    P.op('dve', (lambda n_: (lambda e: e.tensor_reduce(mx[:, 4:5], mx[:, 0:n_], AX.X, ALU.max)))(nb),
                         [K(mx[:])], [K(mx[:])])
                    P.ts('dve', mx[:, 5:6], mx[:, 4:5], -SCALE, None, ALU.mult)
                    for b, (ps, w) in enumerate(banks):
                        P.act(pb[:, b * 512:b * 512 + w], ps[:, 0:w], AF.Exp, bias=mx[:, 5:6], scale=SCALE)
                    P.ts('dve', pb[:, nkeys - 64:nkeys], pb[:, nkeys - 64:nkeys], dmask[:, 0:1], None, ALU.mult)
                    for g4 in range((nk + 3) // 4):
                        pt = next_ps(C)
                        ptb = pt[:].bitcast(BF16)
                        n4 = min(4, nk - g4 * 4)
                        for j in range(n4):
                            kb = g4 * 4 + j
                            P.tr(ptb[:, j * 128:(j + 1) * 128], pb[:, kb * 128:(kb + 1) * 128], C.identb[:])
                        P.copy('act', ptt[:, g4 * 4:g4 * 4 + n4, :],
                               ptb[:, 0:n4 * 128].rearrange("p (j t) -> p j t", j=n4))
                    po = next_ps(C)
                    for kb in range(nk):
                        P.mm(po[:, 0:130], ptt[:, kb, :], Va[:, kb, 0:130], start=(kb == 0), stop=(kb == nk - 1))
                    P.op('dve', (lambda po_: (lambda e: e.reciprocal(mx[:, 6:7], po_[:, 128:129])))(po), [K(po[:])], [K(mx[:])])
                    P.ts('dve', On[:], po[:, 0:128], mx[:, 6:7], None, ALU.mult)
                    pt = next_ps(C)
                    P.tr(pt[:, 0:128], On[:], C.ident[:])
                    P.copy('act', OT[:, h, tsl], pt[:, 0:128])
            P.flush(serialize=True)
            with ExitStack() as st2:
                P.stack = st2
                wo = [P.sb("awo%d" % i, [128, 32, 256], BF16) for i in range(2)]
                xs = [P.sb("axs%d" % i, [128, 256], F32) for i in range(3)]
                osrc = W['mla_w_o'].rearrange("(k p) n -> p k n", p=128)
                n = 0
                for cb in range(16):
                    wb = wo[cb % 2]
                    P.dma('pool', wb[:], osrc[:, :, cb * 256:(cb + 1) * 256])
                    for i in range(NTILE):
                        x_ = xs[n % 3]
                        n += 1
                        P.dma('sp', x_[:], h_d[i * 128:(i + 1) * 128, cb * 256:(cb + 1) * 256])
                        ps = next_ps(C)
                        for k in range(32):
                            P.mm(ps[:, 0:256], OT[:, k, i * 128:(i + 1) * 128], wb[:, k, :], start=(k == 0), stop=(k == 31))
                        P.stt(x_[:], x_[:], ALPHA, ps[:, 0:256], ALU.mult, ALU.add)
                        P.dma('sp', rpre_d[i * 128:(i + 1) * 128, cb * 256:(cb + 1) * 256], x_[:])
                P.flush()
    P.stack = root


def _decl(nc, name, shape, dtype=F32):
    return nc.dram_tensor(name, list(shape), dtype, kind="ExternalInput").ap()


A_INPUTS = [
    ("x_mine", [NT, D]), ("x_prev", [NT, D]),
    ("hy_w_in", [D, 6144]), ("hy_w_out", [D, D]),
    ("gm_w_s", [8, 128, 128]), ("gm_b_s", [8, 128]), ("gm_ln_g", [8, 256]), ("gm_ln_b", [8, 256]),
    ("lamre_B", [128, 16, 128]), ("lamim_B", [128, 16, 128]), ("logdt_B", [128, 16, 128]),
    ("bre_B", [128, 16, 128]), ("bim_B", [128, 16, 128]),
    ("lamre_S", [128, 64]), ("lamim_S", [128, 64]), ("logdt_S", [128, 64]),
    ("cl_S", [128, 64, 2, 32]), ("d_T", [128, 16]),
    ("ssm_w_glu", [2048, 2048]), ("bglu_T", [128, 16]),
]
MOE_INPUTS = [
    ("ln_g", [1, 2, D]), ("ln_b", [1, 2, D]),
    ("moe_w_router", [1, D, NEXP]), ("moe_b_router", [1, NEXP]),
    ("moe_w_gu", [1, NEXP, D, 2048]), ("moe_b_gu_T", [1, NEXP, 128, 16]),
    ("moe_w_down", [1, NEXP, DEXP, D]), ("moe_b_down", [1, NEXP, D]),
]


def build_A(max_flush=None):
    nc = bass.Bass("TRN2", target_bir_lowering=False)
    W = {}
    for name, shape in A_INPUTS + MOE_INPUTS:
        W[name] = _decl(nc, name, shape)
    out = nc.dram_tensor("out", [NT, D], F32, kind="ExternalOutput").ap()
    S = {
        'uT_d': nc.dram_tensor("uT_d", [2048, 2 * NT], BF16).ap(),
        'catT_d': nc.dram_tensor("catT_d", [D, NT], BF16).ap(),
    }
    rpre_d = nc.dram_tensor("rpre_d", [NT, D], F32).ap()
    Y_d = nc.dram_tensor("Y_d", [NT, D], F32).ap()
    with ExitStack() as root:
        P = Prog(nc, root)
        P.max_flush = max_flush
        C = Ctx()
        try:
            setup_common(P, C)
            P.flush()
            phase_l0(P, C, W, W['x_mine'], W['x_prev'], rpre_d, S)
            phase_moe(P, C, W, 0, rpre_d, out, Y_d, "m0")
        except StopBuild:
            P.stack = root
            P.ops = []
        P.finalizing = True
        P.op('sp', lambda e: e.nop(), [], [])
        P.flush(final=True)
    return nc


B_INPUTS = [
    ("h_mine", [NT, D], F32), ("h_prev", [NT, D], F32),
    ("pos_mine", [NT, 1], I32), ("pos_prev", [NT, 1], I32), ("flag", [128, 1], F32),
    ("mla_w_in", [D, 1600], F32), ("mla_q_norm_g", [1, 1024], F32), ("mla_kv_norm_g", [1, 512], F32),
    ("mla_w_uq", [1024, 6144], F32), ("mla_w_ukv", [512, 8192], F32), ("mla_w_o", [D, D], F32),
]


def build_B(max_flush=None):
    nc = bass.Bass("TRN2", target_bir_lowering=False)
    W = {}
    for name, shape, dt_ in B_INPUTS:
        W[name] = _decl(nc, name, shape, dt_)
    for name, shape in MOE_INPUTS:
        W[name] = _decl(nc, name, shape)
    out = nc.dram_tensor("out", [NT, D], F32, kind="ExternalOutput").ap()
    S = {
        'kvT_d': nc.dram_tensor("kvT_d", [576, 2 * NT], BF16).ap(),
        'cqT_d': nc.dram_tensor("cqT_d", [1024, NT], BF16).ap(),
    }
    rpre_d = nc.dram_tensor("rpre_d", [NT, D], F32).ap()
    Y_d = nc.dram_tensor("Y_d", [NT, D], F32).ap()
    with ExitStack() as root:
        P = Prog(nc, root)
        P.max_flush = max_flush
        C = Ctx()
        setup_common(P, C)
        P.flush()
        phase_l1(P, C, W, W['h_mine'], W['h_prev'], rpre_d, S)
        phase_moe(P, C, W, 0, rpre_d, out, Y_d, "m1")
        P.finalizing = True
        P.op('sp', lambda e: e.nop(), [], [])
        P.flush(final=True)
    return nc


def run_B(inp, h0, cores=range(8), max_flush=None):
    f = np.float32
    pos = np.asarray(inp["positions"]).astype(np.int32)
    shared = {
        "mla_w_in": np.asarray(inp["mla_w_in"][0], f),
        "mla_q_norm_g": np.asarray(inp["mla_q_norm_g"], f).reshape(1, 1024),
        "mla_kv_norm_g": np.asarray(inp["mla_kv_norm_g"], f).reshape(1, 512),
        "mla_w_uq": np.asarray(inp["mla_w_uq"][0], f), "mla_w_ukv": np.asarray(inp["mla_w_ukv"][0], f),
        "mla_w_o": np.asarray(inp["mla_w_o"][0], f),
    }
    shared.update(_moe_inputs(inp, 1))
    in_maps = []
    for c in cores:
        b, half = c // 2, c % 2
        m = dict(shared)
        m["h_mine"] = np.ascontiguousarray(h0[b, half * NT:(half + 1) * NT])
        m["h_prev"] = np.ascontiguousarray(h0[b, 0:NT]) if half == 1 else np.zeros((NT, D), f)
        m["pos_mine"] = np.ascontiguousarray(pos[b, half * NT:(half + 1) * NT]).reshape(NT, 1)
        m["pos_prev"] = np.ascontiguousarray(pos[b, 0:NT]).reshape(NT, 1)
        m["flag"] = np.full((128, 1), float(half), f)
        in_maps.append(m)
    nc = build_B(max_flush)
    res = run_bass_kernel_spmd(nc, in_maps, core_ids=list(range(len(in_maps))))
    return [r["out"] for r in res.results]


def _ssm_layouts(inp):
    f = np.float32
    lam_re = np.asarray(inp["ssm_lam_re"][0], f)
    lam_im = np.asarray(inp["ssm_lam_im"][0], f)
    logdt = np.asarray(inp["ssm_log_dt"][0], f)
    b_re = np.asarray(inp["ssm_b_re"][0], f)
    b_im = np.asarray(inp["ssm_b_im"][0], f)
    c_re = np.asarray(inp["ssm_c_re"][0], f)
    c_im = np.asarray(inp["ssm_c_im"][0], f)
    d = np.asarray(inp["ssm_d"][0], f)

    def lay_B(a):
        t = a.reshape(16, 8, 64).transpose(1, 0, 2)
        t = np.broadcast_to(t[:, None, :, None, :], (8, 16, 16, 2, 64))
        return np.ascontiguousarray(t).reshape(128, 16, 128)

    def lay_Bmat(b):
        t = b.reshape(16, 8, 64, 16).transpose(1, 3, 0, 2)
        o = np.zeros((8, 16, 16, 2, 64), f)
        for g8 in range(8):
            o[g8, :, :, g8 % 2, :] = t[g8]
        return o.reshape(128, 16, 128)

    def lay_S(a):
        return np.ascontiguousarray(a.reshape(64, 2, 64).transpose(1, 2, 0)).reshape(128, 64)

    cl = np.zeros((2, 64, 64, 2, 2, 16), f)
    for ri, cm in enumerate((c_re, c_im)):
        t = cm.reshape(64, 2, 16, 64).transpose(1, 3, 0, 2)
        for gl in range(2):
            cl[gl, :, :, ri, gl, :] = t[gl]
    return {
        "lamre_B": lay_B(lam_re), "lamim_B": lay_B(lam_im),
        "logdt_B": lay_B(np.broadcast_to(logdt[:, None], (128, 64))),
        "bre_B": lay_Bmat(b_re), "bim_B": lay_Bmat(b_im),
        "lamre_S": lay_S(lam_re), "lamim_S": lay_S(lam_im),
        "logdt_S": lay_S(np.broadcast_to(logdt[:, None], (128, 64))),
        "cl_S": cl.reshape(128, 64, 2, 32),
        "d_T": np.ascontiguousarray(d.reshape(16, 8, 16).transpose(1, 2, 0)).reshape(128, 16),
    }


def _moe_inputs(inp, L):
    f = np.float32
    return {
        "ln_g": np.asarray(inp["ln_g"][L:L + 1], f), "ln_b": np.asarray(inp["ln_b"][L:L + 1], f),
        "moe_w_router": np.asarray(inp["moe_w_router"][L:L + 1], f),
        "moe_b_router": np.asarray(inp["moe_b_router"][L:L + 1], f),
        "moe_w_gu": np.asarray(inp["moe_w_gu"][L:L + 1], f),
        "moe_b_gu_T": np.ascontiguousarray(
            np.asarray(inp["moe_b_gu"][L:L + 1], f).reshape(1, NEXP, 16, 128).transpose(0, 1, 3, 2)),
        "moe_w_down": np.asarray(inp["moe_w_down"][L:L + 1], f),
        "moe_b_down": np.asarray(inp["moe_b_down"][L:L + 1], f),
    }


def run_A(inp, cores=range(8), max_flush=None):
    f = np.float32
    x = np.asarray(inp["x"], f)
    shared = {
        "hy_w_in": np.asarray(inp["hy_w_in"][0], f), "hy_w_out": np.asarray(inp["hy_w_out"][0], f),
        "gm_w_s": np.asarray(inp["gm_w_s"][0], f), "gm_b_s": np.asarray(inp["gm_b_s"][0], f),
        "gm_ln_g": np.asarray(inp["gm_ln_g"][0], f), "gm_ln_b": np.asarray(inp["gm_ln_b"][0], f),
        "ssm_w_glu": np.asarray(inp["ssm_w_glu"][0], f),
        "bglu_T": np.ascontiguousarray(np.asarray(inp["ssm_b_glu"][0], f).reshape(16, 128).T),
    }
    shared.update(_ssm_layouts(inp))
    shared.update(_moe_inputs(inp, 0))
    in_maps = []
    for c in cores:
        b, half = c // 2, c % 2
        m = dict(shared)
        m["x_mine"] = np.ascontiguousarray(x[b, half * NT:(half + 1) * NT])
        m["x_prev"] = np.ascontiguousarray(x[b, 0:NT]) if half == 1 else np.zeros((NT, D), f)
        in_maps.append(m)
    nc = build_A(max_flush)
    res = run_bass_kernel_spmd(nc, in_maps, core_ids=list(range(len(in_maps))))
    return [r["out"] for r in res.results]


def kernel(**inp):
    outs = run_A(inp)
    h0 = np.stack(outs).reshape(4, 2 * NT, D)
    outs = run_B(inp, h0)
    return np.stack(outs).reshape(4, 2 * NT, D).astype(np.float32)
```
